# Optimizing a Trainium2 kernel written in Bass

```python
import jax, jax.numpy as jnp
from jax import lax
import numpy as np

D_MODEL = 2048
BATCH = 1
SEQ = 8192
DEPTH = 1

CONV_WIDTH = 1024
CONV_KERNEL = 3
ATTN_HEADS = 16
ATTN_HEAD_DIM = 64
ATTN_WIDTH = ATTN_HEADS * ATTN_HEAD_DIM
Q_BLOCK = 128
IN_WIDTH = 3 * CONV_WIDTH + 3 * ATTN_WIDTH + 2 * D_MODEL
PEER_HEADS = 8
PEER_KEYS = 128
PEER_EXPERTS = PEER_KEYS * PEER_KEYS
PEER_QUERY_DIM = 256
PEER_HALF = PEER_QUERY_DIM // 2
PEER_TOPK = 16
PEER_TOKEN_BLOCK = 128
RMS_EPS = 1e-6

kernel_name = "hybrid_conv_stickbreak_peer"


def rmsnorm(x, gain):
    xf = x.astype(jnp.float32)
    inv = lax.rsqrt(jnp.mean(xf * xf, axis=-1, keepdims=True) + RMS_EPS)
    return (xf * inv * gain.astype(jnp.float32)).astype(x.dtype)


def short_conv_mixer(b_gate, c_gate, h, conv_w):
    z = c_gate * h
    y = lax.conv_general_dilated(
        z, conv_w[:, None, :], window_strides=(1,),
        padding=[(CONV_KERNEL - 1, 0)],
        dimension_numbers=("NWC", "WIO", "NWC"),
        feature_group_count=z.shape[-1])
    return b_gate * y


def stick_breaking_attention(q, k, v):
    b, t, h, dh = q.shape
    nb = t // Q_BLOCK
    scale = dh ** -0.5
    kf = k.astype(jnp.float32)
    vf = v.astype(jnp.float32)
    q_blocks = jnp.moveaxis(q.reshape(b, nb, Q_BLOCK, h, dh), 1, 0)
    key_pos = jnp.arange(t)

    def one_block(args):
        qb, blk = args
        q_pos = blk * Q_BLOCK + jnp.arange(Q_BLOCK)
        mask = key_pos[None, :] < q_pos[:, None]
        z = jnp.einsum("bqhd,bkhd->bhqk", qb.astype(jnp.float32), kf) * scale
        log_keep = jnp.where(mask, jax.nn.log_sigmoid(-z), 0.0)
        suffix = lax.cumsum(log_keep, axis=3, reverse=True)
        suffix_excl = jnp.concatenate(
            [suffix[..., 1:], jnp.zeros_like(suffix[..., :1])], axis=-1)
        weights = jnp.where(mask, jnp.exp(jax.nn.log_sigmoid(z) + suffix_excl), 0.0)
        return jnp.einsum("bhqk,bkhd->bqhd", weights, vf)

    out = lax.map(one_block, (q_blocks, jnp.arange(nb)))
    return jnp.moveaxis(out, 0, 1).reshape(b, t, h, dh).astype(q.dtype)


def peer_ffn(x, wq, subkeys, u_table, v_table):
    b, t, d = x.shape
    q = jnp.einsum("btd,dq->btq", x, wq).reshape(b, t, PEER_HEADS, 2, PEER_HALF)
    scores = jnp.einsum("bthsc,snc->bthsn", q, subkeys)
    top_s, top_i = lax.top_k(scores, PEER_TOPK)
    cand_s = top_s[..., 0, :, None] + top_s[..., 1, None, :]
    cand_i = top_i[..., 0, :, None] * PEER_KEYS + top_i[..., 1, None, :]
    cand_s = cand_s.reshape(b, t, PEER_HEADS, PEER_TOPK * PEER_TOPK)
    cand_i = cand_i.reshape(b, t, PEER_HEADS, PEER_TOPK * PEER_TOPK)
    sel_s, sel_pos = lax.top_k(cand_s, PEER_TOPK)
    sel_i = jnp.take_along_axis(cand_i, sel_pos, axis=-1)
    gate = jax.nn.softmax(sel_s.astype(jnp.float32), axis=-1)

    n = b * t
    nb = n // PEER_TOKEN_BLOCK
    xs = x.reshape(nb, PEER_TOKEN_BLOCK, d)
    idx = sel_i.reshape(nb, PEER_TOKEN_BLOCK, PEER_HEADS, PEER_TOPK)
    gs = gate.reshape(nb, PEER_TOKEN_BLOCK, PEER_HEADS, PEER_TOPK)

    def one_block(args):
        xb, ib, gb = args
        u = u_table[ib]
        hidden = jnp.einsum("td,thkd->thk", xb, u)
        act = jax.nn.gelu(hidden.astype(jnp.float32), approximate=False) * gb
        v = v_table[ib]
        return jnp.einsum("thk,thkd->td", act.astype(xb.dtype), v)

    out = lax.map(one_block, (xs, idx, gs))
    return out.reshape(b, t, d)


def setup_inputs(seed: int = 0) -> dict:
    key = jax.random.key(seed)
    ks = jax.random.split(key, 14)
    f32 = jnp.float32
    x = jax.random.normal(ks[0], (BATCH, SEQ, D_MODEL), f32)
    norm_mix = 1.0 + 0.02 * jax.random.normal(ks[1], (DEPTH, D_MODEL), f32)
    w_in = jax.random.normal(ks[2], (DEPTH, D_MODEL, IN_WIDTH), f32) * D_MODEL ** -0.5
    conv_w = jax.random.normal(ks[3], (DEPTH, CONV_KERNEL, CONV_WIDTH), f32) * CONV_KERNEL ** -0.5
    w_conv_out = jax.random.normal(ks[4], (DEPTH, CONV_WIDTH, D_MODEL), f32) * CONV_WIDTH ** -0.5
    w_attn_out = jax.random.normal(ks[5], (DEPTH, ATTN_WIDTH, D_MODEL), f32) * ATTN_WIDTH ** -0.5
    w_out = jax.random.normal(ks[6], (DEPTH, D_MODEL, D_MODEL), f32) * D_MODEL ** -0.5
    norm_ffn = 1.0 + 0.02 * jax.random.normal(ks[7], (DEPTH, D_MODEL), f32)
    peer_wq = jax.random.normal(ks[8], (DEPTH, D_MODEL, PEER_HEADS * PEER_QUERY_DIM), f32) * D_MODEL ** -0.5
    peer_subkeys = jax.random.normal(ks[9], (DEPTH, 2, PEER_KEYS, PEER_HALF), f32) * PEER_HALF ** -0.5
    peer_u = jax.random.normal(ks[10], (DEPTH, PEER_EXPERTS, D_MODEL), f32) * D_MODEL ** -0.5
    peer_v = jax.random.normal(ks[11], (DEPTH, PEER_EXPERTS, D_MODEL), f32) * PEER_HEADS ** -0.5
    norm_final = 1.0 + 0.02 * jax.random.normal(ks[12], (D_MODEL,), f32)
    return {"x": x, "norm_mix": norm_mix, "w_in": w_in, "conv_w": conv_w,
            "w_conv_out": w_conv_out, "w_attn_out": w_attn_out, "w_out": w_out,
            "norm_ffn": norm_ffn, "peer_wq": peer_wq, "peer_subkeys": peer_subkeys,
            "peer_u": peer_u, "peer_v": peer_v, "norm_final": norm_final}


def reference(x, norm_mix, w_in, conv_w, w_conv_out, w_attn_out, w_out,
              norm_ffn, peer_wq, peer_subkeys, peer_u, peer_v, norm_final):
    b, t, d = x.shape
    splits = [CONV_WIDTH, 2 * CONV_WIDTH, 3 * CONV_WIDTH,
              3 * CONV_WIDTH + ATTN_WIDTH, 3 * CONV_WIDTH + 2 * ATTN_WIDTH,
              3 * CONV_WIDTH + 3 * ATTN_WIDTH, 3 * CONV_WIDTH + 3 * ATTN_WIDTH + D_MODEL]
    for layer in range(DEPTH):
        xn = rmsnorm(x, norm_mix[layer])
        proj = jnp.einsum("btd,de->bte", xn, w_in[layer])
        cb, cc, ch, q, k, v, g_conv, g_attn = jnp.split(proj, splits, axis=-1)
        y_conv = short_conv_mixer(cb, cc, ch, conv_w[layer])
        y_conv = jnp.einsum("btc,cd->btd", y_conv, w_conv_out[layer])
        y_attn = stick_breaking_attention(
            q.reshape(b, t, ATTN_HEADS, ATTN_HEAD_DIM),
            k.reshape(b, t, ATTN_HEADS, ATTN_HEAD_DIM),
            v.reshape(b, t, ATTN_HEADS, ATTN_HEAD_DIM)).reshape(b, t, ATTN_WIDTH)
        y_attn = jnp.einsum("bta,ad->btd", y_attn, w_attn_out[layer])
        merged = jax.nn.sigmoid(g_conv) * y_conv + jax.nn.sigmoid(g_attn) * y_attn
        x = x + jnp.einsum("btd,de->bte", merged, w_out[layer])
        xn = rmsnorm(x, norm_ffn[layer])
        x = x + peer_ffn(xn, peer_wq[layer], peer_subkeys[layer], peer_u[layer], peer_v[layer])
    return rmsnorm(x, norm_final)
```

```python
import contextlib
import numpy as np
import concourse.bass as bass
import concourse.mybir as mybir
from concourse.bass_utils import run_bass_kernel_spmd

F32 = mybir.dt.float32
BF16 = mybir.dt.bfloat16
I32 = mybir.dt.int32
U32 = mybir.dt.uint32
ALU = mybir.AluOpType
AF = mybir.ActivationFunctionType
AX = mybir.AxisListType

ENGS = ("pe", "act", "dve", "pool", "sp")
NDMA = 10
EPS = 1e-6
NCORES = 8


class Sched:
    def __init__(self, nc):
        self.nc = nc
        self.ops = []
        self.lastw = {}
        self.readers = {}
        self.fence_from = 0
        self.epoch = 0

    def add(self, eng, fn, r=(), w=(), dma=False, extra_deps=(), cc=False):
        assert not (eng == "pool" and fn is not None and not dma and not cc), "no Pool compute"
        idx = len(self.ops)
        deps = set(extra_deps)
        for k in r:
            if k in self.lastw:
                deps.add(self.lastw[k])
        for k in w:
            if k in self.lastw:
                deps.add(self.lastw[k])
            for q in self.readers.get(k, ()):
                deps.add(q)
        keep = []
        for d in deps:
            o = self.ops[d]
            if o["fn"] is None:
                continue
            if o["eng"] == "pe" and eng == "pe" and not o["dma"] and not dma:
                continue
            o["sig"] = True
            keep.append(d)
        self.ops.append(dict(i=idx, eng=eng, fn=fn, dma=dma, deps=sorted(keep, reverse=True), sig=False,
                             ep=self.epoch))
        for k in r:
            self.readers.setdefault(k, []).append(idx)
        for k in w:
            self.lastw[k] = idx
            self.readers[k] = []
        return idx

    def fence(self, exclude=()):
        last = {}
        dmas = []
        for op in self.ops[self.fence_from:]:
            if op["i"] in exclude or op["fn"] is None:
                continue
            if op["dma"]:
                dmas.append(op["i"])
            else:
                last[op["eng"]] = op["i"]
        deps = list(last.values()) + dmas
        self.fence_from = len(self.ops)
        for e in ENGS:
            self.add(e, None, extra_deps=deps)
        self.epoch += 1

    def emit(self):
        nc = self.nc
        cnt = {}
        rr = {e: 0 for e in ENGS}
        tot = {}
        for op in self.ops:
            e = op["eng"]
            if op["fn"] is None:
                continue
            if op["dma"]:
                slot = rr[e] % NDMA
                rr[e] += 1
                prev = tot.get((e, slot), 0)
                op["sem"] = ("dma", e, slot)
                op["prev"] = prev
                op["val"] = prev + 16
                tot[(e, slot)] = prev + 16
            elif op["sig"]:
                sk = ("eng", e, op["ep"])
                cnt[sk] = cnt.get(sk, 0) + 1
                op["sem"] = sk
                op["val"] = cnt[sk]
        semkeys = sorted(cnt.keys()) + [("dma",) + k for k in sorted(tot.keys())]
        byeng = {e: [op for op in self.ops if op["eng"] == e] for e in ENGS}
        with contextlib.ExitStack() as st:
            sems = {}
            for k in semkeys:
                sems[k] = st.enter_context(nc.semaphore("s_" + "_".join(str(x) for x in k)))
            block = st.enter_context(nc.Block())

            def make(e):
                def body(eng):
                    waited = {}
                    for op in byeng[e]:
                        for d in op["deps"]:
                            o = self.ops[d]
                            sk, v = o["sem"], o["val"]
                            if waited.get(sk, 0) >= v:
                                continue
                            eng.wait_ge(sems[sk], v)
                            waited[sk] = v
                        if op["fn"] is None:
                            continue
                        if op["dma"]:
                            sk = op["sem"]
                            if op["prev"] > 0 and waited.get(sk, 0) < op["prev"]:
                                eng.wait_ge(sems[sk], op["prev"])
                                waited[sk] = op["prev"]
                            op["fn"](eng).then_inc(sems[sk], 16)
                        else:
                            ins = op["fn"](eng)
                            if op["sig"]:
                                ins.then_inc(sems[op["sem"]], 1)
                    for (ee, slot), v in tot.items():
                        if ee == e and waited.get(("dma", ee, slot), 0) < v:
                            eng.wait_ge(sems[("dma", ee, slot)], v)
                return body

            block.tensor(make("pe"))
            block.scalar(make("act"))
            block.vector(make("dve"))
            block.gpsimd(make("pool"))
            block.sync(make("sp"))


SB_BASE = 16512
SB_TOP = 229344
CONST_BYTES = 8192
KB = 1024


def build_program(stage=99):
    nc = bass.Bass("TRN2", target_bir_lowering=False)

    def din(name, shape, dt=F32):
        return nc.dram_tensor(name, shape, dt, kind="ExternalInput").ap()

    x_all = din("x_all", [8192, 2048])
    x_own = din("x_own", [1024, 2048])
    x_halo = din("x_halo", [2, 2048])
    w_qkv = din("w_qkv", [2048, 384])
    w_in = din("w_in", [2048, 10240])
    g_mix = din("g_mix", [1, 2048])
    g_ffn = din("g_ffn", [1, 2048])
    g_fin = din("g_fin", [1, 2048])
    conv_w = din("conv_w", [128, 24])
    w_co = din("w_co", [1024, 2048])
    w_ao = din("w_ao", [1024, 2048])
    w_out = din("w_out", [2048, 2048])
    sel_d = din("sel", [128, 8])
    consts_d = din("consts", [128, 2560])
    wq_d = din("peer_wq", [2048, 2048])
    subk = din("subk", [2, 128, 128])
    pu = din("peer_u", [16384, 2048]) if stage in (4, 7, 10, 99) else None
    pv = din("peer_v", [16384, 2048]) if stage in (4, 10, 99) else None
    out = nc.dram_tensor("out", [1024, 2048], F32, kind="ExternalOutput").ap()
    ib = nc.dram_tensor("ib", [128, 8192], BF16)
    ob = nc.dram_tensor("ob", [1024, 8192], BF16)
    dbg = None
    if stage == 1:
        dbg = nc.dram_tensor("dbg", [3, 128, 8192], BF16, kind="ExternalOutput").ap()
    if stage in (2, 8, 9):
        dbg = nc.dram_tensor("dbg", [128, 8192], BF16, kind="ExternalOutput").ap()
    if stage in (6, 7):
        dbg = nc.dram_tensor("dbg", [128, 16384], BF16, kind="ExternalOutput").ap()
    if stage == 5:
        dbg5 = nc.dram_tensor("dbg", [3, 128, 1024], F32, kind="ExternalOutput").ap()
    if stage in (3, 4):
        dbg = nc.dram_tensor("dbg", [1024, 2048], F32, kind="ExternalOutput").ap()

    S = Sched(nc)
    ps = [nc.alloc_psum_tensor(f"ps{i}", [128, 512], F32) for i in range(8)]

    def PS(b):
        return ("ps", b)

    def esize(dt):
        return 4 if dt in (F32, I32, U32) else 2

    def sb_at(name, shape, dt, off):
        nbytes = int(np.prod(shape[1:])) * esize(dt)
        assert off % 32 == 0, (name, off)
        assert SB_BASE + off + nbytes <= SB_TOP, (name, off, nbytes)
        return nc.alloc_sbuf_tensor_at(name, shape, dt, offset=SB_BASE + off)

    co = [0]

    def cst(name, shape, dt):
        nbytes = int(np.prod(shape[1:])) * esize(dt)
        t = sb_at(name, shape, dt, co[0])
        co[0] += (nbytes + 31) // 32 * 32
        assert co[0] <= CONST_BYTES
        return t

    ident = cst("ident", [128, 128], BF16)
    tri_incl = cst("tri_incl", [128, 128], BF16)
    triC = cst("triC", [128, 128], BF16)
    masks = cst("masks", [128, 4, 512], BF16)
    sel = cst("sel", [128, 8], F32)
    cw = cst("cw", [128, 24], F32)
    ss = cst("ss", [128, 8], F32)
    rstd = cst("rstd", [128, 8], F32)
    identf = cst("identf", [128, 128], F32)
    iota128 = cst("iota128", [128, 128], F32)

    cstage = sb_at("cstage", [128, 2560], F32, CONST_BYTES + 48 * KB)
    S.add("sp", lambda e: e.dma_start(out=cstage[:, :], in_=consts_d[:, :]), w=["cstage"], dma=True)
    S.add("dve", lambda e: e.tensor_copy(out=ident[:, :], in_=cstage[:, 0:128]), r=["cstage"], w=["ident"])
    S.add("dve", lambda e: e.tensor_copy(out=tri_incl[:, :], in_=cstage[:, 128:256]), r=["cstage"], w=["tri_incl"])
    S.add("dve", lambda e: e.tensor_copy(out=triC[:, :], in_=cstage[:, 256:384]), r=["cstage"], w=["triC"])
    S.add("dve", lambda e: e.tensor_copy(out=identf[:, :], in_=cstage[:, 0:128]), r=["cstage"], w=["identf"])
    S.add("dve", lambda e: e.tensor_copy(out=iota128[:, :], in_=cstage[:, 384:512]), r=["cstage"], w=["iota128"])
    S.add("dve", lambda e: e.tensor_copy(out=masks[:, :, :], in_=cstage[:, 512:2560].rearrange("p (r f) -> p r f", r=4)),
          r=["cstage"], w=[("mask", r_) for r_ in range(4)])
    S.add("sp", lambda e: e.dma_start(out=sel[:, :], in_=sel_d[:, :]), w=["sel"], dma=True)
    S.add("sp", lambda e: e.dma_start(out=cw[:, :], in_=conv_w[:, :]), w=["cw"], dma=True)

    B = CONST_BYTES

    ib0 = nc.dram_tensor("ib0", [16, 64], F32)
    ob0 = nc.dram_tensor("ob0", [128, 64], F32)
    if stage != 10:
        S.add("sp", lambda e: e.dma_start(out=ib0[:, :], in_=consts_d[0:16, 0:64]),
              w=["ib0"], dma=True)
        S.add("pool", lambda e: e.collective_compute("AllGather", ALU.bypass, replica_groups=[list(range(NCORES))],
                                                     ins=[ib0.ap().opt()], outs=[ob0.ap().opt()]),
              r=["ib0"], w=["ob0"], cc=True)

    def mm(out_ap, lhsT, rhs, start, stop, r, w):
        S.add("pe", lambda e: e.matmul(out_ap, lhsT=lhsT, rhs=rhs, start=start, stop=stop), r=r, w=w)

    def rms_prep(xt_ap, np_, slot, xs_ap, gb_t, junk_t, rkeys, wkeys):
        ssl = ss[0:np_, slot:slot + 1]
        rsl = rstd[0:np_, slot:slot + 1]
        S.add("dve", lambda e: e.memset(ssl, 0.0), w=[("ss", slot)])
        S.add("act", lambda e: e.activation(out=junk_t[0:np_, :], in_=xt_ap, func=AF.Square,
                                            scale=float(2048 ** -0.5), accum_out=ssl),
              r=rkeys + [("ss", slot)], w=["junk", ("ss", slot)])
        S.add("dve", lambda e: e.tensor_scalar(out=ssl, in0=ssl, scalar1=EPS, scalar2=None, op0=ALU.add),
              r=[("ss", slot)], w=[("ss", slot)])
        S.add("act", lambda e: e.activation(out=rsl, in_=ssl, func=AF.Sqrt), r=[("ss", slot)], w=[("rstd", slot)])
        S.add("dve", lambda e: e.reciprocal(out=rsl, in_=rsl), r=[("rstd", slot)], w=[("rstd", slot)])
        S.add("dve", lambda e: e.scalar_tensor_tensor(out=xs_ap, in0=xt_ap, scalar=rsl, in1=gb_t[0:np_, :],
                                                      op0=ALU.mult, op1=ALU.mult),
              r=rkeys + [("rstd", slot), "gb"], w=wkeys)

    o = B
    QT = sb_at("QT", [128, 8192], BF16, o); o += 16 * KB
    KT = sb_at("KT", [128, 8192], BF16, o); o += 16 * KB
    V = sb_at("V", [128, 64, 128], BF16, o); o += 16 * KB
    yT = sb_at("yT", [128, 8192], BF16, o); o += 16 * KB
    Wqkv = sb_at("Wqkv", [128, 16, 384], BF16, o); o += 12 * KB
    gb = sb_at("gb", [128, 2048], F32, o); o += 8 * KB
    xst = [sb_at(f"xst{i}", [128, 2048], F32, o + i * 8 * KB) for i in range(3)]
    wq_st = sb_at("wq_st", [128, 16, 384], F32, o); o += 24 * KB
    xs = [sb_at(f"xs{i}", [128, 2048], BF16, o + i * 4 * KB) for i in range(2)]; o += 8 * KB
    junk = sb_at("junk", [128, 2048], BF16, o); o += 4 * KB
    xnT = [sb_at(f"xnT{i}", [128, 16, 512], BF16, o + i * 16 * KB) for i in range(2)]; o += 32 * KB
    eb = [sb_at(f"eb{i}", [128, 512], F32, o + i * 2 * KB) for i in range(4)]; o += 8 * KB
    Lb = [sb_at(f"Lb{i}", [128, 512], BF16, o + i * KB) for i in range(6)]; o += 6 * KB
    ePb = [sb_at(f"ePb{i}", [128, 512], F32, o + i * 2 * KB) for i in range(3)]; o += 6 * KB
    wb = [sb_at(f"wb{i}", [128, 512], BF16, o + i * KB) for i in range(4)]; o += 4 * KB

    if stage != 10:
        S.add("sp", lambda e: e.dma_start(out=wq_st[:, :, :], in_=w_qkv.rearrange("(c p) n -> p c n", p=128)),
              w=[("xst", 0), ("xst", 1), ("xst", 2)], dma=True)
        S.add("sp", lambda e: e.dma_start(out=gb[:, :], in_=g_mix.partition_broadcast(128)), w=["gb"], dma=True)
        S.add("dve", lambda e: e.tensor_copy(out=Wqkv[:, :, :], in_=wq_st[:, :, :]),
              r=[("xst", 0), ("xst", 1), ("xst", 2)], w=["Wqkv"])

        for i in range(64):
            k = i % 3
            tl = i % 4
            blk = i // 4
            xb = blk % 2
            S.add("sp", lambda e, i=i, k=k: e.dma_start(out=xst[k][:, :], in_=x_all[i * 128:(i + 1) * 128, :]),
                  w=[("xst", k)], dma=True)
            rms_prep(xst[k][:, :], 128, i % 4, xs[i % 2][:, :], gb, junk, [("xst", k)], [("xs", i % 2)])
            for g in range(4):
                for j in range(4):
                    dc = 4 * g + j
                    mm(ps[g][:, j * 128:(j + 1) * 128], xs[i % 2][:, dc * 128:(dc + 1) * 128], ident[:, :], True, True,
                       [("xs", i % 2), "ident"], [PS(g)])
                dst = xnT[xb][:, 4 * g:4 * g + 4, tl * 128:(tl + 1) * 128]
                src = ps[g][:, :].rearrange("p (a c) -> p a c", a=4)
                if g % 2:
                    S.add("act", lambda e, dst=dst, src=src: e.activation(out=dst, in_=src, func=AF.Copy),
                          r=[PS(g)], w=[("xnT", xb, tl, g)])
                else:
                    S.add("dve", lambda e, dst=dst, src=src: e.tensor_copy(out=dst, in_=src),
                          r=[PS(g)], w=[("xnT", xb, tl, g)])
            if tl == 3:
                allx = [("xnT", xb, a, b_) for a in range(4) for b_ in range(4)]
                for ci in range(2):
                    for dc in range(16):
                        mm(ps[4 + ci][:, :], Wqkv[:, dc, ci * 128:(ci + 1) * 128], xnT[xb][:, dc, :], dc == 0, dc == 15,
                           ["Wqkv"] + allx, [PS(4 + ci)])
                S.add("act", lambda e, blk=blk: e.activation(out=QT[:, blk * 512:(blk + 1) * 512], in_=ps[4][:, :],
                                                             func=AF.Copy, scale=0.125),
                      r=[PS(4)], w=[("QT", blk)])
                S.add("dve", lambda e, blk=blk: e.tensor_copy(out=KT[:, blk * 512:(blk + 1) * 512], in_=ps[5][:, :]),
                      r=[PS(5)], w=[("KT", blk)])
                for t4 in range(4):
                    for dc in range(16):
                        mm(ps[6][:, t4 * 128:(t4 + 1) * 128], xnT[xb][:, dc, t4 * 128:(t4 + 1) * 128],
                           Wqkv[:, dc, 256:384], dc == 0, dc == 15, ["Wqkv"] + allx, [PS(6)])
                S.add("dve", lambda e, blk=blk: e.tensor_copy(out=V[:, blk * 4:blk * 4 + 4, :],
                                                              in_=ps[6][:, :].rearrange("p (a c) -> p a c", a=4)),
                      r=[PS(6)], w=[("V", blk)])

        if stage == 1:
            S.add("sp", lambda e: e.dma_start(out=dbg[0, :, :], in_=QT[:, :]), r=[("QT", b_) for b_ in range(16)], dma=True)
            S.add("sp", lambda e: e.dma_start(out=dbg[1, :, :], in_=KT[:, :]), r=[("KT", b_) for b_ in range(16)], dma=True)
            S.add("sp", lambda e: e.dma_start(out=dbg[2, :, :], in_=V[:, :, :].rearrange("p a c -> p (a c)")),
                  r=[("V", b_) for b_ in range(16)], dma=True)
            S.emit()
            return nc

        S.fence()
        tiles = []
        for qb in range(16):
            for kb in range(4 * qb + 3, -1, -1):
                for h in range(2):
                    tiles.append((qb, kb, h))
        N = len(tiles)

        def first(i):
            qb, kb, h = tiles[i]
            return kb == 4 * qb + 3

        def lastt(i):
            return tiles[i][1] == 0

        def S1(i):
            qb, kb, h = tiles[i]
            hs = slice(h * 64, (h + 1) * 64)
            mm(ps[i % 4][:, :], KT[hs, kb * 128:(kb + 1) * 128], QT[hs, qb * 512:(qb + 1) * 512], True, True,
               [("KT", kb // 4), ("QT", qb)], [PS(i % 4)])

        def S2(i):
            qb, kb, h = tiles[i]
            e_ = eb[i % 4]
            L_ = Lb[i % 6]
            S.add("act", lambda e: e.activation(out=e_[:, :], in_=ps[i % 4][:, :], func=AF.Exp),
                  r=[PS(i % 4)], w=[("eb", i % 4)])
            S.add("act", lambda e: e.activation(out=L_[:, :], in_=e_[:, :], func=AF.Ln, bias=1.0),
                  r=[("eb", i % 4)], w=[("Lb", i % 6)])
            if kb >= 4 * qb:
                r_ = kb - 4 * qb
                S.add("dve", lambda e: e.tensor_tensor(out=L_[:, :], in0=L_[:, :], in1=masks[:, r_, :], op=ALU.mult),
                      r=[("Lb", i % 6), ("mask", r_)], w=[("Lb", i % 6)])

        def S3(i):
            qb, kb, h = tiles[i]
            Pb = ps[4 + h]
            if first(i):
                mm(Pb[:, :], tri_incl[:, :], Lb[i % 6][:, :], True, False, ["tri_incl", ("Lb", i % 6)], [PS(4 + h)])
            else:
                j = i - 2
                mm(Pb[:, :], triC[:, :], Lb[j % 6][:, :], False, False, ["triC", ("Lb", j % 6)], [PS(4 + h)])
                mm(Pb[:, :], tri_incl[:, :], Lb[i % 6][:, :], False, lastt(i), ["tri_incl", ("Lb", i % 6)], [PS(4 + h)])

        def S4(i):
            qb, kb, h = tiles[i]
            eP = ePb[i % 3]
            w_ = wb[i % 4]
            S.add("act", lambda e: e.activation(out=eP[:, :], in_=ps[4 + h][:, :], func=AF.Exp, scale=-1.0),
                  r=[PS(4 + h)], w=[("ePb", i % 3)])
            S.add("dve", lambda e: e.tensor_tensor(out=w_[:, :], in0=eb[i % 4][:, :], in1=eP[:, :], op=ALU.mult),
                  r=[("eb", i % 4), ("ePb", i % 3)], w=[("wb", i % 4)])
            if kb >= 4 * qb:
                r_ = kb - 4 * qb
                S.add("dve", lambda e: e.tensor_tensor(out=w_[:, :], in0=w_[:, :], in1=masks[:, r_, :], op=ALU.mult),
                      r=[("wb", i % 4), ("mask", r_)], w=[("wb", i % 4)])

        def S5(i):
            qb, kb, h = tiles[i]
            Ob = ps[6 + h]
            mm(Ob[:, :], V[:, kb, :], wb[i % 4][:, :], first(i), lastt(i), [("V", kb // 4), ("wb", i % 4)], [PS(6 + h)])
            if lastt(i):
                hs = slice(h * 64, (h + 1) * 64)
                S.add("dve", lambda e: e.tensor_copy(out=yT[hs, qb * 512:(qb + 1) * 512], in_=Ob[hs, :]),
                      r=[PS(6 + h)], w=[("yT", qb, h)])

        for n in (list(range(-2, N)) * (2 if stage == 9 else 1)):
            if 0 <= n + 2 < N:
                S1(n + 2)
            if 0 <= n + 1 < N:
                S2(n + 1)
                S3(n + 1)
            if 0 <= n < N:
                S4(n)
                S5(n)

        ykeys = [("yT", qb, h) for qb in range(16) for h in range(2)]
        if stage in (2, 9):
            S.add("sp", lambda e: e.dma_start(out=dbg[:, :], in_=yT[:, :]), r=ykeys, dma=True)
            S.emit()
            return nc

        S.add("sp", lambda e: e.dma_start(out=ib[:, :], in_=yT[:, :]), r=ykeys, w=["ib"], dma=True)
        cc_op = S.add("pool", lambda e: e.collective_compute("AllGather", ALU.bypass,
                                                             replica_groups=[list(range(NCORES))],
                                                             ins=[ib.ap().opt()], outs=[ob.ap().opt()]),
                      r=["ib"], w=["ob"], cc=True)
        if stage == 8:
            S.add("sp", lambda e: e.dma_start(out=dbg[:, :], in_=ob[256:384, :]), r=["ob"], dma=True)
            S.emit()
            return nc
        S.fence(exclude=[cc_op])

    xnTo = sb_at("xnTo", [128, 16, 1024], BF16, B)
    yconvT = sb_at("yconvT", [128, 8, 1024], BF16, B + 32 * KB)
    yattnT = sb_at("yattnT", [128, 8, 1024], BF16, B + 48 * KB)
    mergedT = sb_at("mergedT", [128, 16, 1024], BF16, B + 64 * KB)
    wst = [sb_at(f"wst{i}", [128, 4096], F32, B + 96 * KB + i * 16 * KB) for i in range(2)]
    wbf = [sb_at(f"wbf{i}", [128, 4096], BF16, B + 128 * KB + i * 8 * KB) for i in range(2)]
    o = B + 144 * KB
    zT = sb_at("zT", [128, 8, 1026], BF16, o)
    o2 = o + 16448
    xst2 = [sb_at(f"xst2_{i}", [128, 2048], F32, o2 + i * 8 * KB) for i in range(2)]; o2 += 16 * KB
    xs2 = [sb_at(f"xs2_{i}", [128, 2048], BF16, o2 + i * 4 * KB) for i in range(2)]; o2 += 8 * KB
    junk2 = sb_at("junk2", [128, 2048], BF16, o2)
    ctmp = [sb_at(f"ctmp{i}", [128, 512], F32, o2 + i * 2 * KB) for i in range(2)]; o2 += 4 * KB
    gb2 = sb_at("gb2", [128, 2048], F32, o2); o2 += 8 * KB
    xnTh = sb_at("xnTh", [128, 16, 2], BF16, o2); o2 += 64

    w_in_v = w_in.rearrange("(c p) n -> p c n", p=128)
    w_co_v = w_co.rearrange("(c p) n -> p c n", p=128)
    w_ao_v = w_ao.rearrange("(c p) n -> p c n", p=128)
    w_out_v = w_out.rearrange("(c p) n -> p c n", p=128)

    wcount = [0]
    castc = [0]

    def cast2a():
        castc[0] += 1
        return "act" if castc[0] % 2 else "dve"

    def wload(src_ap, nk, ncols, cast_eng):
        k = wcount[0] % 2
        wcount[0] += 1
        stv = wst[k][:, 0:nk * ncols].rearrange("p (a c) -> p a c", a=nk)
        bfv = wbf[k][:, 0:nk * ncols].rearrange("p (a c) -> p a c", a=nk)
        S.add("sp", lambda e: e.dma_start(out=stv, in_=src_ap), w=[("wst", k)], dma=True)
        if cast_eng == "pool":
            cast_eng = cast2a()
        if cast_eng == "act":
            S.add("act", lambda e: e.activation(out=bfv, in_=stv, func=AF.Copy), r=[("wst", k)], w=[("wbf", k)])
        else:
            S.add(cast_eng, lambda e: e.tensor_copy(out=bfv, in_=stv), r=[("wst", k)], w=[("wbf", k)])
        return bfv, ("wbf", k)

    bankc = [0]

    def nbank(n=6):
        b_ = bankc[0] % n
        bankc[0] += 1
        return b_

    S.add("sp", lambda e: e.dma_start(out=gb2[:, :], in_=g_mix.partition_broadcast(128)), w=["gb"], dma=True)
    for i in range(9):
        k = i % 2
        np_ = 128 if i < 8 else 2
        src = x_own[i * 128:(i + 1) * 128, :] if i < 8 else x_halo[:, :]
        S.add("sp", lambda e, k=k, np_=np_, src=src: e.dma_start(out=xst2[k][0:np_, :], in_=src),
              w=[("xst", k)], dma=True)
        rms_prep(xst2[k][0:np_, :], np_, i % 4, xs2[k][0:np_, :], gb2, junk2, [("xst", k)], [("xs", k)])
        if i < 8:
            for g in range(4):
                for j in range(4):
                    dc = 4 * g + j
                    mm(ps[g][:, j * 128:(j + 1) * 128], xs2[k][:, dc * 128:(dc + 1) * 128], ident[:, :], True, True,
                       [("xs", k), "ident"], [PS(g)])
                dst = xnTo[:, 4 * g:4 * g + 4, i * 128:(i + 1) * 128]
                src_ = ps[g][:, :].rearrange("p (a c) -> p a c", a=4)
                if g % 2:
                    S.add("act", lambda e, dst=dst, src_=src_: e.activation(out=dst, in_=src_, func=AF.Copy),
                          r=[PS(g)], w=[("xnTo", i, g)])
                else:
                    S.add("dve", lambda e, dst=dst, src_=src_: e.tensor_copy(out=dst, in_=src_),
                          r=[PS(g)], w=[("xnTo", i, g)])
        else:
            for dc in range(16):
                mm(ps[6][:, dc * 2:dc * 2 + 2], xs2[k][0:2, dc * 128:(dc + 1) * 128], ident[0:2, 0:2], True, True,
                   [("xs", k), "ident"], [PS(6)])
            S.add("dve", lambda e: e.tensor_copy(out=xnTh[:, :, :],
                                                 in_=ps[6][:, 0:32].rearrange("p (a c) -> p a c", c=2)),
                  r=[PS(6)], w=["xnTh"])

    def xkeys(half):
        return [("xnTo", i, g) for i in range(4 * half, 4 * half + 4) for g in range(4)]

    def proj(bank, wv, wkey, cs, half):
        for dc in range(16):
            mm(ps[bank][:, :], wv[:, dc, cs], xnTo[:, dc, half * 512:(half + 1) * 512], dc == 0, dc == 15,
               [wkey] + xkeys(half), [PS(bank)])

    def proj_halo(wv, wkey, cs):
        for dc in range(16):
            mm(ps[6][:, 0:2], wv[:, dc, cs], xnTh[:, dc, :], dc == 0, dc == 15, [wkey, "xnTh"], [PS(6)])

    for gi in range(4):
        wv, wk = wload(w_in_v[:, :, 1024 + 256 * gi:1024 + 256 * (gi + 1)], 16, 256, cast2a())
        for cl in range(2):
            j = 2 * gi + cl
            cs = slice(cl * 128, (cl + 1) * 128)
            for half in range(2):
                b_ = nbank()
                proj(b_, wv, wk, cs, half)
                S.add("act", lambda e, b_=b_, j=j, half=half: e.activation(
                    out=zT[:, j, 2 + half * 512:2 + (half + 1) * 512], in_=ps[b_][:, :], func=AF.Copy),
                    r=[PS(b_)], w=[("zT", j)])
            proj_halo(wv, wk, cs)
            S.add("dve", lambda e, j=j: e.tensor_copy(out=zT[:, j, 0:2], in_=ps[6][:, 0:2]), r=[PS(6)], w=[("zT", j)])
    for gi in range(4):
        wv, wk = wload(w_in_v[:, :, 2048 + 256 * gi:2048 + 256 * (gi + 1)], 16, 256, cast2a())
        for cl in range(2):
            j = 2 * gi + cl
            cs = slice(cl * 128, (cl + 1) * 128)
            for half in range(2):
                b_ = nbank()
                proj(b_, wv, wk, cs, half)
                S.add("dve", lambda e, b_=b_, j=j, half=half: e.tensor_tensor(
                    out=zT[:, j, 2 + half * 512:2 + (half + 1) * 512], in0=ps[b_][:, :],
                    in1=zT[:, j, 2 + half * 512:2 + (half + 1) * 512], op=ALU.mult),
                    r=[PS(b_), ("zT", j)], w=[("zT", j)])
            proj_halo(wv, wk, cs)
            S.add("dve", lambda e, j=j: e.tensor_tensor(out=zT[:, j, 0:2], in0=ps[6][:, 0:2], in1=zT[:, j, 0:2],
                                                        op=ALU.mult),
                  r=[PS(6), ("zT", j)], w=[("zT", j)])
    for gi in range(4):
        wv, wk = wload(w_in_v[:, :, 256 * gi:256 * (gi + 1)], 16, 256, cast2a())
        for cl in range(2):
            j = 2 * gi + cl
            cs = slice(cl * 128, (cl + 1) * 128)
            for half in range(2):
                b_ = nbank()
                proj(b_, wv, wk, cs, half)
                t_ = ctmp[half]
                sg = half * 512

                def conv_ops(j=j, sg=sg, t_=t_, b_=b_, half=half):
                    S.add("dve", lambda e: e.tensor_scalar(out=t_[:, :], in0=zT[:, j, 2 + sg:2 + sg + 512],
                                                           scalar1=cw[:, 3 * j + 2:3 * j + 3], scalar2=None,
                                                           op0=ALU.mult),
                          r=[("zT", j), "cw"], w=["junk", ("ctmp", half)])
                    S.add("dve", lambda e: e.scalar_tensor_tensor(out=t_[:, :], in0=zT[:, j, 1 + sg:1 + sg + 512],
                                                                  scalar=cw[:, 3 * j + 1:3 * j + 2], in1=t_[:, :],
                                                                  op0=ALU.mult, op1=ALU.add),
                          r=[("zT", j), "cw", ("ctmp", half)], w=[("ctmp", half)])
                    S.add("dve", lambda e: e.scalar_tensor_tensor(out=t_[:, :], in0=zT[:, j, sg:sg + 512],
                                                                  scalar=cw[:, 3 * j:3 * j + 1], in1=t_[:, :],
                                                                  op0=ALU.mult, op1=ALU.add),
                          r=[("zT", j), "cw", ("ctmp", half)], w=[("ctmp", half)])
                    S.add("dve", lambda e: e.tensor_tensor(out=yconvT[:, j, sg:sg + 512], in0=ps[b_][:, :],
                                                           in1=t_[:, :], op=ALU.mult),
                          r=[PS(b_), ("ctmp", half)], w=[("yconvT", j, half)])
                conv_ops()

    if stage != 10:
        S.fence(exclude=[cc_op])
        Yr = [sb_at(f"Yr{i}", [128, 8192], BF16, B + 144 * KB + 16448 + i * 16 * KB) for i in range(2)]
        for r_ in range(8):
            k = r_ % 2
            S.add("sp", lambda e, r_=r_, k=k: e.dma_start(out=Yr[k][:, :], in_=ob[r_ * 128:(r_ + 1) * 128, :]),
                  r=["ob"], w=[("Yr", k)], dma=True)
            eng = "dve"
            for j in range(8):
                if j == 0:
                    S.add(eng, lambda e, r_=r_, k=k: e.tensor_scalar(out=yattnT[:, r_, :], in0=Yr[k][:, 0:1024],
                                                                     scalar1=sel[:, 0:1], scalar2=None, op0=ALU.mult),
                          r=[("Yr", k), "sel"], w=[("yat", r_)])
                else:
                    S.add(eng, lambda e, r_=r_, k=k, j=j: e.scalar_tensor_tensor(
                        out=yattnT[:, r_, :], in0=Yr[k][:, j * 1024:(j + 1) * 1024], scalar=sel[:, j:j + 1],
                        in1=yattnT[:, r_, :], op0=ALU.mult, op1=ALU.add),
                        r=[("Yr", k), "sel", ("yat", r_)], w=[("yat", r_)])

    S.fence()
    o3 = B + 144 * KB
    sgc = sb_at("sgc", [128, 2, 1024], BF16, o3); o3 += 4 * KB
    sga = sb_at("sga", [128, 2, 1024], BF16, o3); o3 += 4 * KB
    t1 = sb_at("t1", [128, 2, 1024], F32, o3); o3 += 8 * KB
    ut = [sb_at(f"ut{i}", [128, 512], F32, o3 + i * 2 * KB) for i in range(2)]; o3 += 4 * KB
    for pi in range(8):
        for (gname, gt, c0) in (("sgc", sgc, 6144), ("sga", sga, 8192)):
            wv, wk = wload(w_in_v[:, :, c0 + 256 * pi:c0 + 256 * (pi + 1)], 16, 256, "pool")
            for dcl in range(2):
                cs = slice(dcl * 128, (dcl + 1) * 128)
                for half in range(2):
                    b_ = nbank(8)
                    proj(b_, wv, wk, cs, half)
                    S.add("act", lambda e, b_=b_, gt=gt, dcl=dcl, half=half: e.activation(
                        out=gt[:, dcl, half * 512:(half + 1) * 512], in_=ps[b_][:, :], func=AF.Sigmoid),
                        r=[PS(b_)], w=[(gname, dcl, half)])
        wv, wk = wload(w_co_v[:, :, 256 * pi:256 * (pi + 1)], 8, 256, "pool")
        for dcl in range(2):
            cs = slice(dcl * 128, (dcl + 1) * 128)
            for half in range(2):
                b_ = nbank(8)
                for c8 in range(8):
                    mm(ps[b_][:, :], wv[:, c8, cs], yconvT[:, c8, half * 512:(half + 1) * 512], c8 == 0, c8 == 7,
                       [wk, ("yconvT", c8, half)], [PS(b_)])
                S.add("dve", lambda e, b_=b_, dcl=dcl, half=half: e.tensor_tensor(
                    out=t1[:, dcl, half * 512:(half + 1) * 512], in0=ps[b_][:, :],
                    in1=sgc[:, dcl, half * 512:(half + 1) * 512], op=ALU.mult),
                    r=[PS(b_), ("sgc", dcl, half)], w=[("t1", dcl, half)])
        wv, wk = wload(w_ao_v[:, :, 256 * pi:256 * (pi + 1)], 8, 256, "pool")
        for dcl in range(2):
            cs = slice(dcl * 128, (dcl + 1) * 128)
            for half in range(2):
                b_ = nbank(8)
                for c8 in range(8):
                    mm(ps[b_][:, :], wv[:, c8, cs], yattnT[:, c8, half * 512:(half + 1) * 512], c8 == 0, c8 == 7,
                       [wk, ("yat", c8)], [PS(b_)])
                u_ = ut[half]
                S.add("dve", lambda e, b_=b_, dcl=dcl, half=half, u_=u_: e.tensor_tensor(
                    out=u_[:, :], in0=ps[b_][:, :], in1=sga[:, dcl, half * 512:(half + 1) * 512], op=ALU.mult),
                    r=[PS(b_), ("sga", dcl, half)], w=[("ut", half)])
                S.add("dve", lambda e, pi=pi, dcl=dcl, half=half, u_=u_: e.tensor_tensor(
                    out=mergedT[:, 2 * pi + dcl, half * 512:(half + 1) * 512], in0=u_[:, :],
                    in1=t1[:, dcl, half * 512:(half + 1) * 512], op=ALU.add),
                    r=[("ut", half), ("t1", dcl, half)], w=[("mT", 2 * pi + dcl, half)])

    S.fence()
    x2 = sb_at("x2", [128, 8, 2048], F32, B)
    for i in range(8):
        S.add("sp", lambda e, i=i: e.dma_start(out=x2[:, i, :], in_=x_own[i * 128:(i + 1) * 128, :]),
              w=[("x2", i)], dma=True)
    for gi in range(8):
        wv, wk = wload(w_out_v[:, :, 256 * gi:256 * (gi + 1)], 16, 256, "pool")
        for i in range(8):
            b_ = nbank(8)
            half = i // 4
            for dm in range(16):
                mm(ps[b_][:, 0:256], mergedT[:, dm, i * 128:(i + 1) * 128], wv[:, dm, :], dm == 0, dm == 15,
                   [wk] + [("mT", d_, half) for d_ in range(16)], [PS(b_)])
            S.add("dve", lambda e, b_=b_, i=i, gi=gi: e.tensor_tensor(
                out=x2[:, i, 256 * gi:256 * (gi + 1)], in0=ps[b_][:, 0:256],
                in1=x2[:, i, 256 * gi:256 * (gi + 1)], op=ALU.add),
                r=[PS(b_), ("x2", i)], w=[("x2", i)])

    if stage == 3:
        for i in range(8):
            S.add("sp", lambda e, i=i: e.dma_start(out=dbg[i * 128:(i + 1) * 128, :], in_=x2[:, i, :]),
                  r=[("x2", i)], dma=True)
        S.emit()
        return nc

    x2_d = nc.dram_tensor("x2_d", [1024, 2048], F32)
    Gd = nc.dram_tensor("Gd", [8, 128, 128, 128], BF16)
    actT_d = nc.dram_tensor("actT_d", [128, 128, 1024], BF16)
    wq_v = wq_d.rearrange("(c p) n -> p c n", p=128)

    xn2T = sb_at("xn2T", [128, 16, 1024], BF16, B + 64 * KB)
    gb3 = sb_at("gb3", [128, 2048], F32, B + 144 * KB)
    xs3 = [sb_at(f"xs3_{i}", [128, 2048], BF16, B + 152 * KB + i * 4 * KB) for i in range(2)]
    junk3 = sb_at("junk3", [128, 2048], BF16, B + 160 * KB)
    S.add("sp", lambda e: e.dma_start(out=gb3[:, :], in_=g_ffn.partition_broadcast(128)), w=["gb"], dma=True)
    for i in range(8):
        k = i % 2
        rms_prep(x2[:, i, :], 128, i % 4, xs3[k][:, :], gb3, junk3, [("x2", i)], [("xs", k)])
        for g in range(4):
            for j in range(4):
                dc = 4 * g + j
                mm(ps[g][:, j * 128:(j + 1) * 128], xs3[k][:, dc * 128:(dc + 1) * 128], ident[:, :], True, True,
                   [("xs", k), "ident"], [PS(g)])
            dst = xn2T[:, 4 * g:4 * g + 4, i * 128:(i + 1) * 128]
            src_ = ps[g][:, :].rearrange("p (a c) -> p a c", a=4)
            if g % 2:
                S.add("act", lambda e, dst=dst, src_=src_: e.activation(out=dst, in_=src_, func=AF.Copy),
                      r=[PS(g)], w=[("xn2T", i, g)])
            else:
                S.add("dve", lambda e, dst=dst, src_=src_: e.tensor_copy(out=dst, in_=src_),
                      r=[PS(g)], w=[("xn2T", i, g)])
        S.add("sp", lambda e, i=i: e.dma_start(out=x2_d[i * 128:(i + 1) * 128, :], in_=x2[:, i, :]),
              r=[("x2", i)], w=[("x2d", i)], dma=True)
    S.fence()

    def x2keys(half):
        return [("xn2T", i, g) for i in range(4 * half, 4 * half + 4) for g in range(4)]

    qT = sb_at("qT", [128, 16, 1024], BF16, B)
    ipT = sb_at("ipT", [128, 1024], F32, B + 32 * KB)
    jpT = sb_at("jpT", [128, 1024], F32, B + 36 * KB)
    gT = sb_at("gT", [128, 1024], F32, B + 40 * KB)
    o4 = B + 44 * KB
    skn = sb_at("skn", [128, 2, 128], F32, o4); o4 += KB
    skT = sb_at("skT", [128, 2, 128], BF16, o4); o4 += 512
    tops = sb_at("tops", [128, 16, 16], F32, o4); o4 += KB
    idxu = sb_at("idxu", [128, 16, 16], U32, o4); o4 += KB
    idxf = sb_at("idxf", [128, 16, 16], F32, o4); o4 += KB
    sels = sb_at("sels", [128, 8, 16], F32, o4); o4 += 512
    posu = sb_at("posu", [128, 8, 16], U32, o4); o4 += 512
    posf = sb_at("posf", [128, 8, 16], F32, o4); o4 += 512
    bpf = sb_at("bpf", [128, 8, 16], F32, o4); o4 += 512
    apf = sb_at("apf", [128, 8, 16], F32, o4); o4 += 512
    eg = sb_at("eg", [128, 8, 16], F32, o4); o4 += 512
    gpk = sb_at("gpk", [128, 8, 16], F32, o4); o4 += 512
    ipf = sb_at("ipf", [128, 8, 16], F32, o4); o4 += 512
    jpf = sb_at("jpf", [128, 8, 16], F32, o4); o4 += 512
    Zs = sb_at("Zs", [128, 8], F32, o4); o4 += 32
    eq = sb_at("eq", [128, 8, 16, 16], F32, B + 54 * KB)
    sc = sb_at("sc", [128, 16, 128], F32, B + 144 * KB)
    sc2 = sb_at("sc2", [128, 16, 128], F32, B + 152 * KB)
    cand = sb_at("cand", [128, 8, 256], F32, B + 160 * KB)
    cand2 = sb_at("cand2", [128, 8, 256], F32, B + 168 * KB)

    for gi in range(8):
        wv, wk = wload(wq_v[:, :, 256 * gi:256 * (gi + 1)], 16, 256, "pool")
        for cl in range(2):
            hs = 2 * gi + cl
            cs = slice(cl * 128, (cl + 1) * 128)
            for half in range(2):
                b_ = nbank(8)
                for dc in range(16):
                    mm(ps[b_][:, :], wv[:, dc, cs], xn2T[:, dc, half * 512:(half + 1) * 512], dc == 0, dc == 15,
                       [wk] + x2keys(half), [PS(b_)])
                S.add("act", lambda e, b_=b_, hs=hs, half=half: e.activation(
                    out=qT[:, hs, half * 512:(half + 1) * 512], in_=ps[b_][:, :], func=AF.Copy),
                    r=[PS(b_)], w=[("qT", hs, half)])

    S.add("sp", lambda e: e.dma_start(out=skn[:, :, :], in_=subk.rearrange("s n c -> n s c")), w=["skn"], dma=True)
    for s_ in range(2):
        b_ = nbank(8)
        mm(ps[b_][:, 0:128], skn[:, s_, :], identf[:, :], True, True, ["skn", "identf"], [PS(b_)])
        S.add("act", lambda e, b_=b_, s_=s_: e.activation(out=skT[:, s_, :], in_=ps[b_][:, 0:128], func=AF.Copy),
              r=[PS(b_)], w=["skT"])

    NEG = -1.0e30
    D = lambda fn, r, w: S.add("dve", fn, r=r, w=w)
    for i in range(8):
        for hs in range(16):
            mm(ps[hs // 4][:, (hs % 4) * 128:(hs % 4 + 1) * 128], qT[:, hs, i * 128:(i + 1) * 128], skT[:, hs % 2, :],
               True, True, [("qT", hs, i // 4), "skT"], [PS(hs // 4)])
        for g in range(4):
            S.add("act", lambda e, g=g: e.activation(out=sc[:, 4 * g:4 * g + 4, :],
                                                     in_=ps[g][:, :].rearrange("p (a c) -> p a c", a=4), func=AF.Copy),
                  r=[PS(g)], w=[("sc", g)])
        for hs in range(16):
            g = hs // 4
            D(lambda e, hs=hs: e.max(out=tops[:, hs, 0:8], in_=sc[:, hs, :]), [("sc", g)], ["tops"])
            D(lambda e, hs=hs: e.max_index(out=idxu[:, hs, 0:8], in_max=tops[:, hs, 0:8], in_values=sc[:, hs, :]),
              [("sc", g), "tops"], ["idxu"])
            D(lambda e, hs=hs: e.match_replace(out=sc2[:, hs, :], in_to_replace=tops[:, hs, 0:8],
                                               in_values=sc[:, hs, :], imm_value=NEG),
              [("sc", g), "tops"], ["sc2"])
            D(lambda e, hs=hs: e.max(out=tops[:, hs, 8:16], in_=sc2[:, hs, :]), ["sc2"], ["tops"])
            D(lambda e, hs=hs: e.max_index(out=idxu[:, hs, 8:16], in_max=tops[:, hs, 8:16], in_values=sc2[:, hs, :]),
              ["sc2", "tops"], ["idxu"])
        t4 = tops[:, :, :].rearrange("p (h s) k -> p h s k", s=2)
        c4 = cand[:, :, :].rearrange("p h (a b) -> p h a b", a=16)
        sh4 = [128, 8, 16, 16]
        D(lambda e: e.tensor_tensor(out=c4, in0=t4[:, :, 0, :].unsqueeze(3).broadcast_to(sh4),
                                    in1=t4[:, :, 1, :].unsqueeze(2).broadcast_to(sh4), op=ALU.add),
          ["tops"], ["cand"])
        for h in range(8):
            D(lambda e, h=h: e.max(out=sels[:, h, 0:8], in_=cand[:, h, :]), ["cand"], ["sels"])
            D(lambda e, h=h: e.max_index(out=posu[:, h, 0:8], in_max=sels[:, h, 0:8], in_values=cand[:, h, :]),
              ["cand", "sels"], ["posu"])
            D(lambda e, h=h: e.match_replace(out=cand2[:, h, :], in_to_replace=sels[:, h, 0:8],
                                             in_values=cand[:, h, :], imm_value=NEG), ["cand", "sels"], ["cand2"])
            D(lambda e, h=h: e.max(out=sels[:, h, 8:16], in_=cand2[:, h, :]), ["cand2"], ["sels"])
            D(lambda e, h=h: e.max_index(out=posu[:, h, 8:16], in_max=sels[:, h, 8:16], in_values=cand2[:, h, :]),
              ["cand2", "sels"], ["posu"])
        sh3 = [128, 8, 16]
        D(lambda e: e.tensor_tensor(out=eg[:, :, :], in0=sels[:, :, :], in1=sels[:, :, 0:1].broadcast_to(sh3),
                                    op=ALU.subtract), ["sels"], ["eg"])
        S.add("act", lambda e: e.activation(out=eg[:, :, :], in_=eg[:, :, :], func=AF.Exp), r=["eg"], w=["eg"])
        D(lambda e: e.tensor_reduce(out=Zs[:, :], in_=eg[:, :, :], axis=AX.X, op=ALU.add), ["eg"], ["Zs"])
        D(lambda e: e.reciprocal(out=Zs[:, :], in_=Zs[:, :]), ["Zs"], ["Zs"])
        D(lambda e: e.tensor_tensor(out=gpk[:, :, :], in0=eg[:, :, :], in1=Zs[:, :].unsqueeze(2).broadcast_to(sh3),
                                    op=ALU.mult), ["eg", "Zs"], ["gpk"])
        D(lambda e: e.tensor_single_scalar(out=posf[:, :, :].bitcast(U32), in_=posu[:, :, :], scalar=4,
                                           op=ALU.logical_shift_right), ["posu"], ["posf"])
        D(lambda e: e.tensor_copy(out=apf[:, :, :], in_=posf[:, :, :].bitcast(U32)), ["posf"], ["apf"])
        D(lambda e: e.tensor_single_scalar(out=posf[:, :, :].bitcast(U32), in_=posu[:, :, :], scalar=15,
                                           op=ALU.bitwise_and), ["posu", "apf"], ["posf"])
        D(lambda e: e.tensor_copy(out=bpf[:, :, :], in_=posf[:, :, :].bitcast(U32)), ["posf"], ["bpf"])
        D(lambda e: e.tensor_copy(out=idxf[:, :, :], in_=idxu[:, :, :]), ["idxu"], ["idxf"])
        i4 = idxf[:, :, :].rearrange("p (h s) k -> p h s k", s=2)
        io4 = iota128[:, 0:16].unsqueeze(1).unsqueeze(1).broadcast_to(sh4)
        for (posx, s_, dstf, nm) in ((apf, 0, ipf, "ipf"), (bpf, 1, jpf, "jpf")):
            D(lambda e, posx=posx: e.tensor_tensor(out=eq[:, :, :, :], in0=io4,
                                                   in1=posx[:, :, :].unsqueeze(3).broadcast_to(sh4), op=ALU.is_equal),
              ["iota128", "apf", "bpf"], ["eq"])
            D(lambda e, s_=s_: e.tensor_tensor(out=eq[:, :, :, :], in0=eq[:, :, :, :],
                                               in1=i4[:, :, s_, :].unsqueeze(2).broadcast_to(sh4), op=ALU.mult),
              ["eq", "idxf"], ["eq"])
            D(lambda e, dstf=dstf: e.tensor_reduce(out=dstf[:, :, :], in_=eq[:, :, :, :], axis=AX.X, op=ALU.add),
              ["eq"], [nm])
        for (arr, dstT, nm) in ((ipf, ipT, "ipf"), (jpf, jpT, "jpf"), (gpk, gT, "gpk")):
            b_ = 4 + nbank(4)
            mm(ps[b_][:, 0:128], arr[:, :, :].rearrange("p h k -> p (h k)"), identf[:, :], True, True,
               [nm, "identf"], [PS(b_)])
            S.add("act", lambda e, b_=b_, dstT=dstT, i=i: e.activation(out=dstT[:, i * 128:(i + 1) * 128],
                                                                       in_=ps[b_][:, 0:128], func=AF.Copy),
                  r=[PS(b_)], w=[(nm + "T", i)])

    if stage == 5:
        S.add("sp", lambda e: e.dma_start(out=dbg5[0], in_=ipT[:, :]), r=[("ipfT", i) for i in range(8)], dma=True)
        S.add("sp", lambda e: e.dma_start(out=dbg5[1], in_=jpT[:, :]), r=[("jpfT", i) for i in range(8)], dma=True)
        S.add("sp", lambda e: e.dma_start(out=dbg5[2], in_=gT[:, :]), r=[("gpkT", i) for i in range(8)], dma=True)
        S.emit()
        return nc

    S.fence()
    A1 = sb_at("A1", [128, 128, 128], BF16, B + 96 * KB)
    B1 = sb_at("B1", [128, 128, 128], BF16, B + 128 * KB)
    Gst = [sb_at("Gst0", [128, 128, 128], BF16, B + 160 * KB), sb_at("Gst1", [128, 128, 128], BF16, B)]
    sh = [128, 128, 128]
    io3 = iota128[:, :].unsqueeze(1).broadcast_to(sh)
    for i in range(8):
        k = i % 2
        ts = slice(i * 128, (i + 1) * 128)
        S.add("dve", lambda e, ts=ts: e.tensor_tensor(out=B1[:, :, :], in0=io3,
                                                      in1=jpT[:, ts].unsqueeze(2).broadcast_to(sh), op=ALU.is_equal),
              r=[("jpfT", i), "iota128"], w=["B1"])
        S.add("dve", lambda e, ts=ts: e.tensor_tensor(out=A1[:, :, :], in0=io3,
                                                      in1=ipT[:, ts].unsqueeze(2).broadcast_to(sh), op=ALU.is_equal),
              r=[("ipfT", i), "iota128"], w=["A1"])
        S.add("dve", lambda e, ts=ts: e.tensor_tensor(out=A1[:, :, :], in0=A1[:, :, :],
                                                       in1=gT[:, ts].unsqueeze(2).broadcast_to(sh), op=ALU.mult),
              r=["A1", ("gpkT", i)], w=["A1"])
        for tq in range(32):
            b_ = nbank(8)
            for tt in range(4):
                t_ = 4 * tq + tt
                mm(ps[b_][:, tt * 128:(tt + 1) * 128], B1[:, t_, :], A1[:, t_, :], True, True, ["A1", "B1"], [PS(b_)])
            S.add("act", lambda e, b_=b_, k=k, tq=tq: e.activation(
                out=Gst[k][:, :, 4 * tq:4 * tq + 4], in_=ps[b_][:, :].rearrange("p (t i) -> p i t", t=4),
                func=AF.Copy), r=[PS(b_)], w=[("Gst", k)])
        S.add("sp", lambda e, i=i, k=k: e.dma_start(out=Gd[i], in_=Gst[k][:, :, :]), r=[("Gst", k)], w=[("Gd", i)],
              dma=True)

    if stage == 6:
        S.add("sp", lambda e: e.dma_start(out=dbg[:, :], in_=Gd[5].rearrange("j i t -> j (i t)")),
              r=[("Gd", 5)], dma=True)
        S.emit()
        return nc

    S.fence()
    Gg = [sb_at(f"Gg{i}", [128, 8, 16, 128], BF16, B + i * 32 * KB) for i in range(2)]
    ust = [sb_at(f"ust{i}", [128, 2048], F32, B + 96 * KB + i * 8 * KB) for i in range(2)]
    ubf = [sb_at(f"ubf{i}", [128, 2048], BF16, B + 112 * KB + i * 4 * KB) for i in range(2)]
    uT = [sb_at(f"uT{i}", [128, 16, 128], BF16, B + 120 * KB + i * 4 * KB) for i in range(2)]
    hg = [sb_at(f"hg{i}", [128, 1024], F32, B + 128 * KB + i * 4 * KB) for i in range(2)]
    aT = [sb_at(f"aT{i}", [128, 1024], BF16, B + 136 * KB + i * 2 * KB) for i in range(2)]
    NCH = 128

    def loadu(c):
        k = c % 2
        S.add("sp", lambda e: e.dma_start(out=ust[k][:, :], in_=pu[c * 128:(c + 1) * 128, :]), w=[("ust", k)], dma=True)
        S.add("act", lambda e: e.activation(out=ubf[k][:, 0:1024], in_=ust[k][:, 0:1024], func=AF.Copy),
              r=[("ust", k)], w=[("ubf", k, 0)])
        S.add("dve", lambda e: e.tensor_copy(out=ubf[k][:, 1024:2048], in_=ust[k][:, 1024:2048]),
              r=[("ust", k)], w=[("ubf", k, 1)])

    loadu(0)
    for c in range(NCH):
        g = c // 16
        il = c % 16
        k = c % 2
        if il == 0:
            S.add("sp", lambda e, g=g: e.dma_start(out=Gg[g % 2][:, :, :, :],
                                                   in_=Gd[:, :, 16 * g:16 * (g + 1), :].rearrange("a j i t -> j a i t")),
                  r=[("Gd", a) for a in range(8)], w=[("Gg", g % 2)], dma=True)
        if c + 1 < NCH:
            loadu(c + 1)
        for g4 in range(4):
            for j in range(4):
                dc = 4 * g4 + j
                mm(ps[g4][:, j * 128:(j + 1) * 128], ubf[k][:, dc * 128:(dc + 1) * 128], ident[:, :], True, True,
                   [("ubf", k, g4 // 2), "ident"], [PS(g4)])
            dst = uT[k][:, 4 * g4:4 * g4 + 4, :]
            src_ = ps[g4][:, :].rearrange("p (a c) -> p a c", a=4)
            if g4 % 2:
                S.add("act", lambda e, dst=dst, src_=src_: e.activation(out=dst, in_=src_, func=AF.Copy),
                      r=[PS(g4)], w=[("uT", k, g4)])
            else:
                S.add("dve", lambda e, dst=dst, src_=src_: e.tensor_copy(out=dst, in_=src_),
                      r=[PS(g4)], w=[("uT", k, g4)])
        for half in range(2):
            b_ = 4 + (2 * c + half) % 4
            for dc in range(16):
                mm(ps[b_][:, :], uT[k][:, dc, :], xn2T[:, dc, half * 512:(half + 1) * 512], dc == 0, dc == 15,
                   [("uT", k, dc // 4)] + x2keys(half), [PS(b_)])
            S.add("act", lambda e, b_=b_, k=k, half=half: e.activation(
                out=hg[k][:, half * 512:(half + 1) * 512], in_=ps[b_][:, :], func=AF.Gelu),
                r=[PS(b_)], w=[("hg", k, half)])
            S.add("dve", lambda e, k=k, half=half, g=g, il=il: e.tensor_tensor(
                out=aT[k][:, half * 512:(half + 1) * 512].rearrange("p (a t) -> p a t", a=4),
                in0=hg[k][:, half * 512:(half + 1) * 512].rearrange("p (a t) -> p a t", a=4),
                in1=Gg[g % 2][:, 4 * half:4 * half + 4, il, :], op=ALU.mult),
                r=[("hg", k, half), ("Gg", g % 2)], w=[("aT", k, half)])
        S.add("sp", lambda e, c=c, k=k: e.dma_start(out=actT_d[c], in_=aT[k][:, :]),
              r=[("aT", k, 0), ("aT", k, 1)], w=[("actT_d", c)], dma=True)

    if stage == 7:
        for c in range(16):
            S.add("sp", lambda e, c=c: e.dma_start(out=dbg[:, c * 1024:(c + 1) * 1024], in_=actT_d[8 * c + 3]),
                  r=[("actT_d", 8 * c + 3)], dma=True)
        S.emit()
        return nc

    S.fence()
    x3 = sb_at("x3", [128, 8, 2048], F32, B)
    aTl = [sb_at(f"aTl{i}", [128, 1024], BF16, B + 64 * KB + i * 2 * KB) for i in range(3)]
    vst = [sb_at(f"vst{i}", [128, 512], F32, B + 72 * KB + i * 2 * KB) for i in range(3)]
    vbf = [sb_at(f"vbf{i}", [128, 512], BF16, B + 80 * KB + i * KB) for i in range(3)]
    x2s = [sb_at(f"x2s{i}", [128, 512], F32, B + 84 * KB + i * 2 * KB) for i in range(2)]
    gb4 = sb_at("gb4", [128, 2048], F32, B + 96 * KB)
    junk4 = sb_at("junk4", [128, 2048], BF16, B + 104 * KB)
    ot = [sb_at(f"ot{i}", [128, 2048], F32, B + 112 * KB + i * 8 * KB) for i in range(2)]
    S.add("sp", lambda e: e.dma_start(out=gb4[:, :], in_=g_fin.partition_broadcast(128)), w=["gb"], dma=True)
    for p4 in range(4):
        cs4 = slice(p4 * 512, (p4 + 1) * 512)
        for c in range(NCH):
            k = c % 3
            S.add("sp", lambda e, c=c, k=k: e.dma_start(out=aTl[k][:, :], in_=actT_d[c]),
                  r=[("actT_d", c)], w=[("aTl", k)], dma=True)
            S.add("sp", lambda e, c=c, k=k, cs4=cs4: e.dma_start(out=vst[k][:, :], in_=pv[c * 128:(c + 1) * 128, cs4]),
                  w=[("vst", k)], dma=True)
            S.add("dve", lambda e, k=k: e.tensor_copy(out=vbf[k][:, :], in_=vst[k][:, :]),
                  r=[("vst", k)], w=[("vbf", k)])
            for i in range(8):
                mm(ps[i][:, :], aTl[k][:, i * 128:(i + 1) * 128], vbf[k][:, :], c == 0, c == NCH - 1,
                   [("aTl", k), ("vbf", k)], [PS(i)])
        for i in range(8):
            S.add("sp", lambda e, i=i, cs4=cs4: e.dma_start(out=x2s[i % 2][:, :], in_=x2_d[i * 128:(i + 1) * 128, cs4]),
                  r=[("x2d", i)], w=[("x2s", i % 2)], dma=True)
            S.add("dve", lambda e, i=i, cs4=cs4: e.tensor_tensor(out=x3[:, i, cs4], in0=ps[i][:, :],
                                                                 in1=x2s[i % 2][:, :], op=ALU.add),
                  r=[PS(i), ("x2s", i % 2)], w=[("x3", i, p4)])

    if stage == 4:
        for i in range(8):
            S.add("sp", lambda e, i=i: e.dma_start(out=dbg[i * 128:(i + 1) * 128, :], in_=x3[:, i, :]),
                  r=[("x3", i, p4) for p4 in range(4)], dma=True)
        S.emit()
        return nc

    for i in range(8):
        rms_prep(x3[:, i, :], 128, i % 4, ot[i % 2][:, :], gb4, junk4, [("x3", i, p4) for p4 in range(4)],
                 [("ot", i % 2)])
        S.add("sp", lambda e, i=i: e.dma_start(out=out[i * 128:(i + 1) * 128, :], in_=ot[i % 2][:, :]),
              r=[("ot", i % 2)], dma=True)
    S.emit()
    return nc


def prep_inputs(inputs, cores=range(NCORES), stage=99):
    f = lambda a: np.ascontiguousarray(np.asarray(a, dtype=np.float32))
    x = f(inputs["x"])[0]
    w_in = f(inputs["w_in"])[0]
    cwr = f(inputs["conv_w"])[0]
    conv_w = np.ascontiguousarray(cwr.reshape(3, 8, 128).transpose(2, 1, 0).reshape(128, 24))
    common = {
        "x_all": x, "w_in": w_in,
        "g_mix": f(inputs["norm_mix"]).reshape(1, 2048), "g_ffn": f(inputs["norm_ffn"]).reshape(1, 2048),
        "g_fin": f(inputs["norm_final"]).reshape(1, 2048), "conv_w": conv_w,
        "w_co": f(inputs["w_conv_out"])[0], "w_ao": f(inputs["w_attn_out"])[0], "w_out": f(inputs["w_out"])[0],
        "peer_wq": f(inputs["peer_wq"])[0], "subk": f(inputs["peer_subkeys"])[0],
        "peer_u": f(inputs["peer_u"])[0], "peer_v": f(inputs["peer_v"])[0],
    }
    if stage not in (4, 7, 10, 99):
        del common["peer_u"]
    if stage not in (4, 10, 99):
        del common["peer_v"]
    p_ = np.arange(128)[:, None]
    f_ = np.arange(128)[None, :]
    f5 = np.arange(512)[None, :]
    cparts = [(p_ == f_), (p_ >= f_), (p_ < f_), np.broadcast_to(f_, (128, 128))]
    cparts += [(f5 - p_ - 128 * r_ > 0) for r_ in range(4)]
    common["consts"] = np.ascontiguousarray(np.concatenate([a.astype(np.float32) for a in cparts], axis=1))
    maps = []
    for c in cores:
        m = dict(common)
        m["x_own"] = np.ascontiguousarray(x[c * 1024:(c + 1) * 1024])
        m["x_halo"] = np.ascontiguousarray(x[c * 1024 - 2:c * 1024]) if c > 0 else np.zeros((2, 2048), np.float32)
        cols = []
        for part in range(3):
            base = 3072 + part * 1024 + c * 128
            cols.append(w_in[:, base:base + 128])
        m["w_qkv"] = np.ascontiguousarray(np.concatenate(cols, axis=1))
        selv = np.zeros((128, 8), np.float32)
        selv[:, c] = 1.0
        m["sel"] = selv
        maps.append(m)
    return maps


_NC_CACHE = {}


def kernel(**inputs):
    if "nc" not in _NC_CACHE:
        _NC_CACHE["nc"] = build_program()
    nc = _NC_CACHE["nc"]
    maps = prep_inputs(inputs)
    res = run_bass_kernel_spmd(nc, maps, core_ids=list(range(NCORES)))
    outs = [np.asarray(res.results[c]["out"], dtype=np.float32) for c in range(NCORES)]
    return np.concatenate(outs, axis=0).reshape(1, 8192, 2048)
```
